# Optimizing a Trainium2 kernel written in Bass

```python
import jax, jax.numpy as jnp
from jax import lax
import numpy as np

D_MODEL = 1024
BATCH = 32
SEQ = 2048
DEPTH = 4

CHUNK = 64
N_MIXERS = 2
D_FF = 256 * ((8 * D_MODEL // 3 + 255) // 256)
RMS_EPS = 1e-6
L2_EPS = 1e-6
SC_WIDTH = 3
GDN_HEAD_DIM = 128
GDN_K_HEADS = D_MODEL // 128
GDN_V_HEADS = 2 * GDN_K_HEADS
GDN_KEY_DIM = GDN_K_HEADS * GDN_HEAD_DIM
GDN_VAL_DIM = GDN_V_HEADS * GDN_HEAD_DIM
GDN_CONV_WIDTH = 4
GDN_CONV_DIM = 2 * GDN_KEY_DIM + GDN_VAL_DIM
GDN_IN_DIM = GDN_CONV_DIM + GDN_VAL_DIM + 2 * GDN_V_HEADS
N_SC_LAYERS = (DEPTH + 1) // 2
N_GDN_LAYERS = DEPTH // 2
N_NORMS = 6

kernel_name = "hybrid_shortconv_gdn_macaron_trunk"


def rms_norm(x, g):
    xf = x.astype(jnp.float32)
    y = xf * lax.rsqrt(jnp.mean(xf * xf, axis=-1, keepdims=True) + RMS_EPS)
    return (y * g.astype(jnp.float32)).astype(x.dtype)


def causal_depthwise_conv(x, w):
    k, c = w.shape
    return lax.conv_general_dilated(
        x, w[:, None, :].astype(x.dtype), window_strides=(1,), padding=[(k - 1, 0)],
        dimension_numbers=("NWC", "WIO", "NWC"), feature_group_count=c)


def swiglu(x, w_gate_up, w_down):
    gate, up = jnp.split(x @ w_gate_up, 2, axis=-1)
    return (jax.nn.silu(gate) * up) @ w_down


def short_conv_mixer(x, w_in, conv_w, w_out):
    b, c, v = jnp.split(x @ w_in, 3, axis=-1)
    y = b * causal_depthwise_conv(c * v, conv_w)
    return y @ w_out


def l2norm(x):
    return x * lax.rsqrt(jnp.sum(x * x, axis=-1, keepdims=True) + L2_EPS)


def chunk_gated_delta_rule(q, k, v, g, beta):
    bsz, nh, t, dk = q.shape
    dv = v.shape[-1]
    n = t // CHUNK
    q = q * (dk ** -0.5)

    def rs(a):
        return a.reshape(bsz, nh, n, CHUNK, *a.shape[3:])

    q, k, v, g, beta = rs(q), rs(k), rs(v), rs(g), rs(beta)
    g = jnp.cumsum(g, axis=-1)
    k_beta = k * beta[..., None]
    v_beta = v * beta[..., None]
    idx = jnp.arange(CHUNK)
    causal = idx[:, None] >= idx[None, :]
    strict = idx[:, None] > idx[None, :]
    decay = jnp.exp(jnp.where(causal, g[..., :, None] - g[..., None, :], -jnp.inf))
    a = jnp.where(strict, jnp.einsum("bhncd,bhnsd->bhncs", k_beta, k) * decay, 0.0)
    eye = jnp.eye(CHUNK, dtype=q.dtype)
    t_inv = lax.linalg.triangular_solve(eye + a, jnp.broadcast_to(eye, a.shape),
                                        left_side=True, lower=True)
    u = jnp.einsum("bhncs,bhnse->bhnce", t_inv, v_beta)
    w = jnp.einsum("bhncs,bhnsd->bhncd", t_inv, k_beta * jnp.exp(g)[..., None])
    qk = jnp.where(causal, jnp.einsum("bhncd,bhnsd->bhncs", q, k) * decay, 0.0)
    q_g = q * jnp.exp(g)[..., None]
    g_last = g[..., -1]
    k_tail = k * jnp.exp(g_last[..., None] - g)[..., None]

    def step(state, inp):
        q_g_n, qk_n, u_n, w_n, k_tail_n, gl_n = inp
        v_new = u_n - jnp.einsum("bhcd,bhde->bhce", w_n, state)
        o = jnp.einsum("bhcd,bhde->bhce", q_g_n, state) + jnp.einsum("bhcs,bhse->bhce", qk_n, v_new)
        state = state * jnp.exp(gl_n)[..., None, None] + jnp.einsum("bhcd,bhce->bhde", k_tail_n, v_new)
        return state, o

    xs = tuple(jnp.moveaxis(a_, 2, 0) for a_ in (q_g, qk, u, w, k_tail, g_last))
    s0 = jnp.zeros((bsz, nh, dk, dv), dtype=q.dtype)
    _, o = lax.scan(step, s0, xs)
    return jnp.moveaxis(o, 0, 2).reshape(bsz, nh, t, dv)


def gated_deltanet_mixer(x, w_in, conv_w, a_log, dt_bias, norm_w, w_out):
    bsz, t, _ = x.shape
    proj = x @ w_in
    qkv, z, b, a = jnp.split(proj, [GDN_CONV_DIM, GDN_CONV_DIM + GDN_VAL_DIM,
                                    GDN_CONV_DIM + GDN_VAL_DIM + GDN_V_HEADS], axis=-1)
    qkv = jax.nn.silu(causal_depthwise_conv(qkv, conv_w))
    q, k, v = jnp.split(qkv.astype(jnp.float32), [GDN_KEY_DIM, 2 * GDN_KEY_DIM], axis=-1)
    rep = GDN_V_HEADS // GDN_K_HEADS
    q = jnp.repeat(l2norm(q.reshape(bsz, t, GDN_K_HEADS, GDN_HEAD_DIM)), rep, axis=2)
    k = jnp.repeat(l2norm(k.reshape(bsz, t, GDN_K_HEADS, GDN_HEAD_DIM)), rep, axis=2)
    v = v.reshape(bsz, t, GDN_V_HEADS, GDN_HEAD_DIM)
    beta = jax.nn.sigmoid(b.astype(jnp.float32))
    g = -jnp.exp(a_log.astype(jnp.float32)) * jax.nn.softplus(
        a.astype(jnp.float32) + dt_bias.astype(jnp.float32))
    o = chunk_gated_delta_rule(q.transpose(0, 2, 1, 3), k.transpose(0, 2, 1, 3),
                               v.transpose(0, 2, 1, 3), g.transpose(0, 2, 1),
                               beta.transpose(0, 2, 1))
    o = o.transpose(0, 2, 1, 3)
    o = o * lax.rsqrt(jnp.mean(o * o, axis=-1, keepdims=True) + RMS_EPS) * norm_w.astype(jnp.float32)
    o = o * jax.nn.silu(z.astype(jnp.float32).reshape(bsz, t, GDN_V_HEADS, GDN_HEAD_DIM))
    return o.reshape(bsz, t, GDN_VAL_DIM).astype(x.dtype) @ w_out


def setup_inputs(seed: int = 0) -> dict:
    key = jax.random.key(seed)
    ks = jax.random.split(key, 16)
    f32 = jnp.float32
    d = D_MODEL
    x = jax.random.normal(ks[0], (BATCH, SEQ, d), f32)
    norm_g = 1.0 + 0.02 * jax.random.normal(ks[1], (DEPTH, N_NORMS, d), f32)
    ffn_w_gate_up = jax.random.normal(ks[2], (DEPTH, 2, d, 2 * D_FF), f32) * d ** -0.5
    ffn_w_down = jax.random.normal(ks[3], (DEPTH, 2, D_FF, d), f32) * D_FF ** -0.5
    sc_w_in = jax.random.normal(ks[4], (N_SC_LAYERS, d, 3 * d), f32) * d ** -0.5
    sc_conv_w = jax.random.normal(ks[5], (N_SC_LAYERS, SC_WIDTH, d), f32) * SC_WIDTH ** -0.5
    sc_w_out = jax.random.normal(ks[6], (N_SC_LAYERS, d, d), f32) * d ** -0.5
    gdn_w_in = jax.random.normal(ks[7], (N_GDN_LAYERS, d, GDN_IN_DIM), f32) * d ** -0.5
    gdn_conv_w = jax.random.normal(ks[8], (N_GDN_LAYERS, GDN_CONV_WIDTH, GDN_CONV_DIM), f32) * GDN_CONV_WIDTH ** -0.5
    gdn_a_log = jnp.log(jax.random.uniform(ks[9], (N_GDN_LAYERS, GDN_V_HEADS), f32, 1.0, 16.0))
    gdn_dt_bias = 1.0 + 0.1 * jax.random.normal(ks[10], (N_GDN_LAYERS, GDN_V_HEADS), f32)
    gdn_norm_w = 1.0 + 0.02 * jax.random.normal(ks[11], (N_GDN_LAYERS, GDN_HEAD_DIM), f32)
    gdn_w_out = jax.random.normal(ks[12], (N_GDN_LAYERS, GDN_VAL_DIM, d), f32) * GDN_VAL_DIM ** -0.5
    return {"x": x, "norm_g": norm_g, "ffn_w_gate_up": ffn_w_gate_up, "ffn_w_down": ffn_w_down,
            "sc_w_in": sc_w_in, "sc_conv_w": sc_conv_w, "sc_w_out": sc_w_out,
            "gdn_w_in": gdn_w_in, "gdn_conv_w": gdn_conv_w, "gdn_a_log": gdn_a_log,
            "gdn_dt_bias": gdn_dt_bias, "gdn_norm_w": gdn_norm_w, "gdn_w_out": gdn_w_out}


def reference(x, norm_g, ffn_w_gate_up, ffn_w_down, sc_w_in, sc_conv_w, sc_w_out,
              gdn_w_in, gdn_conv_w, gdn_a_log, gdn_dt_bias, gdn_norm_w, gdn_w_out):
    h = x
    for i in range(DEPTH):
        g = norm_g[i]
        ff = swiglu(rms_norm(h, g[0]), ffn_w_gate_up[i, 0], ffn_w_down[i, 0])
        h = h + 0.5 * rms_norm(ff, g[1])
        m_in = rms_norm(h, g[2])
        j = i // N_MIXERS
        if i % N_MIXERS == 0:
            mix = short_conv_mixer(m_in, sc_w_in[j], sc_conv_w[j], sc_w_out[j])
        else:
            mix = gated_deltanet_mixer(m_in, gdn_w_in[j], gdn_conv_w[j], gdn_a_log[j],
                                       gdn_dt_bias[j], gdn_norm_w[j], gdn_w_out[j])
        h = h + rms_norm(mix, g[3])
        ff = swiglu(rms_norm(h, g[4]), ffn_w_gate_up[i, 1], ffn_w_down[i, 1])
        h = h + 0.5 * rms_norm(ff, g[5])
    return h
```

```python
import numpy as np
from contextlib import ExitStack
import concourse.bass as bass
import concourse.mybir as mybir
from concourse.bass_utils import run_bass_kernel_spmd

F32 = mybir.dt.float32
BF16 = mybir.dt.bfloat16
AF = mybir.ActivationFunctionType
ALU = mybir.AluOpType

D = 1024
NCH = 8
SEQ = 2048
TT = 512
NTT = SEQ // TT
DFF = 2816
NFC = DFF // 128
DEPTH = 4
RMS_EPS = 1e-6
SLOTW = 3072
NSLOT = 4
ARENA = 114 * 1024


GRAN = 512


class Buf:
    __slots__ = ("w", "r", "excl")

    def __init__(self, excl=False):
        self.w = None
        self.r = {}
        self.excl = excl


class Store:
    def __init__(self, h, nbytes, gran=GRAN, excl=False):
        self.h = h
        self.gran = gran
        self.bufs = [Buf(excl) for _ in range((nbytes + gran - 1) // gran)]

    def view(self, off, nbytes, dt=F32, parts=128):
        assert off % 4 == 0 and nbytes % 4 == 0
        ap = self.h[0:parts, off // 4:(off + nbytes) // 4]
        if dt != F32:
            ap = ap.bitcast(dt)
        g = self.gran
        return View(ap, self.bufs[off // g:(off + nbytes - 1) // g + 1])


class View:
    def __init__(self, ap, bufs):
        self.ap = ap
        self.bufs = bufs
        self.sem = None
        self.nload = 0
        self.ssem = None

    def __getitem__(self, idx):
        return self.ap[idx]

    def sub(self, c0, c1):
        esz = 4 if self.ap.dtype == F32 else 2
        n = len(self.bufs)
        g0 = (c0 * esz) // GRAN
        g1 = (c1 * esz - 1) // GRAN + 1
        v = View(self.ap[:, c0:c1], self.bufs[g0:min(g1, n)] if n > 1 else self.bufs)
        return v


class Eng:
    def __init__(self, K, name, e, sem):
        self.K, self.name, self.e, self.sem = K, name, e, sem
        self.n = 0
        self.seen = {}

    def need(self, dep):
        s, v = dep
        if self.seen.get(s, 0) < v:
            if s is self.sem and self.name == "pe":
                return
            self.e.wait_ge(s, v)
            self.seen[s] = v

    def _deps(self, reads, writes):
        for t in reads:
            for b in t.bufs:
                if b.w is not None:
                    self.need(b.w)
                if b.excl:
                    for s, v in b.r.items():
                        if s is not self.sem:
                            self.need((s, v))
        for t in writes:
            for b in t.bufs:
                if b.w is not None:
                    self.need(b.w)
                for s, v in b.r.items():
                    self.need((s, v))

    def op(self, fn, reads=(), writes=()):
        if self.K.dry:
            return None
        self._deps(reads, writes)
        ins = fn(self.e)
        self.n += 1
        ins.then_inc(self.sem, 1)
        me = (self.sem, self.n)
        for t in reads:
            for b in t.bufs:
                b.r[self.sem] = self.n
        for t in writes:
            for b in t.bufs:
                b.w = me
                b.r = {}
        return ins

    def dma(self, out_ap, in_ap, reads=(), writes=(), sem=None):
        if self.K.dry:
            return None
        self._deps(reads, writes)
        ins = self.e.dma_start(out=out_ap, in_=in_ap)
        for t in writes:
            t.nload += 1
            ins.then_inc(t.sem, 16)
            for b in t.bufs:
                b.w = (t.sem, 16 * t.nload)
                b.r = {}
        if reads:
            sem[1] += 1
            ins.then_inc(sem[0], 16)
            for t in reads:
                for b in t.bufs:
                    b.r[sem[0]] = 16 * sem[1]
        return ins


class WeightStream:
    def __init__(self, K):
        self.K = K
        self.plan = []
        self.pos = 0
        self.issued = 0

    def _issue(self, n):
        K = self.K
        slot = K.wslots[n % NSLOT]
        K.pool._deps((), [slot])
        for (dst_ap_fn, src_ap) in self.plan[n]:
            ins = K.pool.e.dma_start(out=dst_ap_fn(slot), in_=src_ap)
            slot.nload += 1
            ins.then_inc(slot.sem, 16)
        for b in slot.bufs:
            b.w = (slot.sem, 16 * slot.nload)
            b.r = {}

    def next(self, specs):
        K = self.K
        if K.dry:
            self.plan.append(specs)
            return K.wslots[0]
        n = self.pos
        self.pos += 1
        while self.issued < min(len(self.plan), n + NSLOT):
            self._issue(self.issued)
            self.issued += 1
        return K.wslots[n % NSLOT]


class Builder:
    def __init__(self, nseq, layers, io_hT=False):
        self.nseq = nseq
        self.layers = layers
        self.dry = False
        self.nc = bass.Bass("TRN2", target_bir_lowering=False)
        self.es = ExitStack()

    def sb(self, name, nbytes, dt=F32, dma=False, parts=128):
        h = self.es.enter_context(self.nc.sbuf_tensor(name, [128, nbytes // 4], F32))
        v = Store(h, nbytes).view(0, nbytes, dt, parts)
        if dma:
            v.sem = self.es.enter_context(self.nc.semaphore("s_" + name))
        return v

    def ps(self, name):
        h = self.es.enter_context(self.nc.psum_tensor(name, [128, 512], F32))
        return Store(h, 2048, gran=2048, excl=True)

    def al(self, nbytes, dt=F32, parts=128):
        al_ = 256 if nbytes <= 256 else GRAN
        off = (self.aoff + al_ - 1) // al_ * al_
        self.aoff = off + nbytes
        assert self.aoff <= ARENA, (self.aoff, ARENA)
        return self.A.view(off, nbytes, dt, parts)

    def build(self):
        nc = self.nc
        nseq = self.nseq
        dt = nc.dram_tensor
        self.x = dt("x", [nseq, SEQ, D], F32, kind="ExternalInput").ap()
        self.out = dt("out", [nseq, SEQ, D], F32, kind="ExternalOutput").ap()
        self.cvec = dt("cvec", [128, CV_W], F32, kind="ExternalInput").ap()
        self.cmat = dt("cmat", [128, CM_W], F32, kind="ExternalInput").ap()
        self.wgu = dt("ffn_w_gate_up", [DEPTH, 2, D, 2 * DFF], F32, kind="ExternalInput").ap()
        self.wdn = dt("ffn_w_down", [DEPTH, 2, DFF, D], F32, kind="ExternalInput").ap()
        self.scin = dt("sc_w_in", [2, D, 3 * D], F32, kind="ExternalInput").ap()
        self.scout = dt("sc_w_out", [2, D, D], F32, kind="ExternalInput").ap()
        self.gdin = dt("gdn_w_in", [2, D, 6176], F32, kind="ExternalInput").ap()
        self.gdout = dt("gdn_w_out", [2, 2048, D], F32, kind="ExternalInput").ap()
        with self.es:
            self._alloc()
            self.dry = True
            self.ws = WeightStream(self)
            self._program()
            self.dry = False
            self._program()
        return nc

    def _alloc(self):
        nc = self.nc
        sem = lambda n: self.es.enter_context(nc.semaphore(n))
        self.pe = Eng(self, "pe", nc.tensor, sem("pe"))
        self.act = Eng(self, "act", nc.scalar, sem("act"))
        self.dve = Eng(self, "dve", nc.vector, sem("dve"))
        self.pool = Eng(self, "pool", nc.gpsimd, sem("pool"))
        self.sp = Eng(self, "sp", nc.sync, sem("sp"))
        self.h = [[self.sb(f"h{c}_{t}", TT * 4) for t in range(NTT)] for c in range(NCH)]
        self.Pst = [self.ps(f"P{i}") for i in range(8)]
        self.P = [p.view(0, 2048) for p in self.Pst]
        self.wslots = [self.sb(f"w{i}", SLOTW * 2, BF16, dma=True) for i in range(NSLOT)]
        self.cv = self.sb("cvec_sb", CV_W * 4, dma=True)
        self.cm = self.sb("cmat_sb", CM_W * 4, dma=True)
        self.cmb = self.sb("cmatb", 256 * 2, BF16)
        self.ghalf = self.sb("ghalf", DEPTH * 6 * NCH * 4)
        ah = self.es.enter_context(nc.sbuf_tensor("arena", [128, ARENA // 4], F32))
        self.A = Store(ah, ARENA)
        self.aoff = 0
        self.xs = [self.al(D * 4) for i in range(4)]
        for i, t in enumerate(self.xs):
            t.sem = sem(f"xld{i}")
            t.ssem = [sem(f"ost{i}"), 0]
        self.aoff = 0
        self.xn = [self.al(NCH * TT * 2, BF16) for i in range(2)]
        self.sqb = [self.al(TT * 2, BF16) for i in range(2)]
        self.rstd = [self.al(TT * 4) for i in range(2)]
        self.ff = [[self.al(TT * 4) for t in range(2)] for j in range(NCH)]
        base = self.aoff
        self.sg = [self.al(TT * 2, BF16) for i in range(2)]
        self.hid = [[self.al(TT * 2, BF16) for t in range(2)] for i in range(NFC)]
        self.aoff = base
        self.cvb = [self.al((TT + 2) * 4) for j in range(NCH)]
        self.csb = [self.al(TT * 4) for i in range(2)]
        self.acc = [self.al(TT * 4) for i in range(2)]
        self.ysc = self.al(NCH * TT * 2, BF16)
        self.aoff = base
        self._alloc_gdn()

    def _alloc_gdn(self):
        K = self
        al = K.al
        spare = [K.ff[j][1] for j in range(NCH)]
        K.g_acc = [spare[0], spare[1]]
        K.g_oT = [spare[2], spare[3]]
        K.g_expgb = spare[4]
        K.g_rinv = spare[5]
        K.g_expgbj = [spare[4], spare[6]]
        K.g_rinvj = [spare[5], spare[7]]
        base_x = 8192
        K.g_rows = [K.A.view(base_x + i * 2048, 2048, F32, parts=32) for i in range(4)]
        K.g_scal = al(192 * 4)
        K.g_nscal = al(192 * 4)
        K.g_scal2 = al(192 * 4)
        K.g_eglb = al(64 * 4)
        K.g_R = al(64 * 4, parts=32)
        K.g_egl = al(16 * 4, parts=32)
        K.g_pre = [al((TT + 3) * 4) for i in range(2)]
        K.g_qT = al(TT * 2, BF16)
        K.g_kT = al(TT * 2, BF16)
        K.g_qg = al(TT * 2, BF16)
        K.g_qgj = [K.g_qg, al(TT * 2, BF16)]
        K.g_vT = [al(TT * 2, BF16) for i in range(2)]
        K.g_zs = [al(TT * 2, BF16) for i in range(2)]
        K.g_og = [al(TT * 2, BF16) for i in range(16)]
        K.g_S = [al(128 * 4) for i in range(16)]
        K.g_Sb = [al(128 * 2, BF16) for i in range(16)]
        K.g_halo = al(32 * 3 * 4)
        f = lambda: al(128 * 4)
        b = lambda: al(128 * 2, BF16)
        mk = lambda: dict(y1=f(), Dst=f(), y2=f(), Dti=f(), nA0=b(), nM0=b(), qkT=b(), B=al(256 * 2, BF16), ktl=b(),
                          Ak=[b(), b()], Mk=[b(), b()], Xbf=[al(256 * 2, BF16), al(256 * 2, BF16)], u=f(), wbf=b(), wT=b(),
                          vnew=b())
        K.g_wj = [mk(), mk()]
        K.g_w = K.g_wj[0]
        K.nexpalog = K.sb("nexpalog", 16 * 4)

    def ident_f(self):
        return self.cm[:, CM_IDENT:CM_IDENT + 128]

    def ident_b(self):
        return self.cmb[:, CM_IDENT:CM_IDENT + 128]

    def ones_b(self):
        return self.cmb[:, CM_ONES:CM_ONES + 128]

    def gcol(self, l, n, c, half=False):
        k = (l * 6 + n) * NCH + c
        if half:
            return self.ghalf[:, k:k + 1]
        return self.cv[:, CV_G + k:CV_G + k + 1]

    def prologue(self):
        K = self
        if True:
            K.sp.dma(K.cv[:, :], K.cvec, writes=[K.cv])
            K.sp.dma(K.cm[:, :], K.cmat, writes=[K.cm])
            K.dve.op(lambda e: e.tensor_copy(out=K.cmb[:, :], in_=K.cm[:, 0:256]), reads=[K.cm], writes=[K.cmb])
            K.dve.op(lambda e: e.tensor_scalar(out=K.ghalf[:, :], in0=K.cv[:, CV_G:CV_G + DEPTH * 6 * NCH],
                                               scalar1=0.5, scalar2=None, op0=ALU.mult),
                     reads=[K.cv], writes=[K.ghalf])

    def epilogue(self):
        K = self
        for t in K.xs:
            K.sp.e.wait_ge(t.ssem[0], 16 * t.ssem[1])

    def _program(self):
        K = self
        if not K.dry:
            K.prologue()
        for s in range(K.nseq):
            K.load_x(s)
            for l in K.layers:
                K.ffn(l, 0)
                if l % 2 == 0:
                    K.shortconv(l)
                else:
                    K.gdn(l)
                K.ffn(l, 1)
            K.store_out(s)
        if not K.dry:
            K.epilogue()

    def load_x(self, s):
        K = self
        for tt in range(NTT):
            for r in range(4):
                K.sp.dma(K.xs[r][:, :], K.x[s, tt * TT + r * 128: tt * TT + (r + 1) * 128, :], writes=[K.xs[r]])
            for c in range(NCH):
                pb = K.P[c % 4]
                for r in range(4):
                    K.pe.op(lambda e: e.matmul(pb[:, r * 128:(r + 1) * 128], lhsT=K.xs[r][:, c * 128:(c + 1) * 128],
                                               rhs=K.ident_f(), start=True, stop=True),
                            reads=[K.xs[r], K.cm], writes=[pb])
                eng = K.act if c % 2 == 0 else K.dve
                if c % 2 == 0:
                    K.act.op(lambda e: e.activation(out=K.h[c][tt][:, :], in_=pb[:, :], func=AF.Copy),
                             reads=[pb], writes=[K.h[c][tt]])
                else:
                    K.dve.op(lambda e: e.tensor_copy(out=K.h[c][tt][:, :], in_=pb[:, :]),
                             reads=[pb], writes=[K.h[c][tt]])

    def store_out(self, s):
        K = self
        for tt in range(NTT):
            for r in range(4):
                st = K.xs[r]
                for half in range(2):
                    pb = K.P[(r * 2 + half) % 4]
                    for cc in range(4):
                        c = half * 4 + cc
                        K.pe.op(lambda e: e.matmul(pb[:, cc * 128:(cc + 1) * 128],
                                                   lhsT=K.h[c][tt][:, r * 128:(r + 1) * 128],
                                                   rhs=K.ident_f(), start=True, stop=True),
                                reads=[K.h[c][tt], K.cm], writes=[pb])
                    if half == 0:
                        K.act.op(lambda e: e.activation(out=st[:, 0:512], in_=pb[:, :], func=AF.Copy),
                                 reads=[pb], writes=[st])
                    else:
                        K.dve.op(lambda e: e.tensor_copy(out=st[:, 512:1024], in_=pb[:, :]),
                                 reads=[pb], writes=[st])
                K.sp.dma(K.out[s, tt * TT + r * 128: tt * TT + (r + 1) * 128, :], st[:, :], reads=[st], sem=st.ssem)

    def rstd_from(self, srcs, slot):
        K = self
        pss = K.P[7]
        for c, (t, ap) in enumerate(srcs):
            sq = K.sqb[c % 2]
            K.act.op(lambda e: e.activation(out=sq[:, :], in_=ap, func=AF.Square), reads=[t], writes=[sq])
            K.pe.op(lambda e: e.matmul(pss[:, :], lhsT=K.ones_b(), rhs=sq[:, :], start=(c == 0), stop=(c == NCH - 1)),
                    reads=[sq, K.cmb], writes=[pss])
        r = K.rstd[slot]
        K.act.op(lambda e: e.activation(out=r[:, :], in_=pss[:, :], func=AF.Sqrt, scale=1.0 / D, bias=K.cv[:, CV_EPS:CV_EPS + 1]),
                 reads=[pss, K.cv], writes=[r])
        K.dve.op(lambda e: e.reciprocal(out=r[:, :], in_=r[:, :]), reads=[r], writes=[r])
        return r

    def prenorm(self, l, n, tt, slot):
        K = self
        r = K.rstd_from([(K.h[c][tt], K.h[c][tt][:, :]) for c in range(NCH)], slot)
        xn = K.xn[slot]
        for c in range(NCH):
            K.dve.op(lambda e: e.scalar_tensor_tensor(out=xn[:, c * TT:(c + 1) * TT], in0=K.h[c][tt][:, :],
                                                      scalar=K.gcol(l, n, c), in1=r[:, :],
                                                      op0=ALU.mult, op1=ALU.mult),
                     reads=[K.h[c][tt], r, K.cv], writes=[xn])
        return xn

    def postnorm_add(self, l, n, tt, srcs, slot, half):
        K = self
        r = K.rstd_from([(t, t[:, :]) for t in srcs], slot)
        lvl = getattr(K, 'dbg_pl', 2)
        for c in range(NCH if lvl >= 1 else 0):
            t = srcs[c]
            K.dve.op(lambda e: e.scalar_tensor_tensor(out=t[:, :], in0=t[:, :], scalar=K.gcol(l, n, c, half=half),
                                                      in1=r[:, :], op0=ALU.mult, op1=ALU.mult),
                     reads=[t, r, K.cv, K.ghalf], writes=[t])
            if lvl < 2:
                continue
            K.dve.op(lambda e: e.tensor_tensor(out=K.h[c][tt][:, :], in0=K.h[c][tt][:, :], in1=t[:, :], op=ALU.add),
                     reads=[t, K.h[c][tt]], writes=[K.h[c][tt]])

    @staticmethod
    def _dst(off, nk, w):
        return lambda slot: slot[:, off:off + nk * w].rearrange("p (k f) -> p k f", k=nk)

    def wspec(self, W2d, col0, ncol, off, nk):
        src = W2d[:, col0:col0 + ncol].rearrange("(k p) f -> p k f", p=128)
        return (self._dst(off, nk, ncol), src)

    def ffn(self, l, a):
        K = self
        Wgu = K.wgu[l, a]
        Wdn = K.wdn[l, a]
        n_pre, n_post = (0, 1) if a == 0 else (4, 5)
        for hf in range(2):
            tts = [2 * hf, 2 * hf + 1]
            xns = [K.prenorm(l, n_pre, tt, i) for i, tt in enumerate(tts)]
            for i in range(getattr(K, 'dbg_nfc', NFC)):
                wt = K.ws.next([K.wspec(Wgu, i * 128, 128, 0, NCH), K.wspec(Wgu, DFF + i * 128, 128, NCH * 128, NCH)])
                for ti in range(2):
                    pg, pu = K.P[ti], K.P[2 + ti]
                    for c in range(NCH):
                        K.pe.op(lambda e: e.matmul(pg[:, :], lhsT=wt[:, c * 128:(c + 1) * 128],
                                                   rhs=xns[ti][:, c * TT:(c + 1) * TT], start=(c == 0), stop=(c == NCH - 1)),
                                reads=[wt, xns[ti]], writes=[pg])
                    for c in range(NCH):
                        K.pe.op(lambda e: e.matmul(pu[:, :], lhsT=wt[:, (NCH + c) * 128:(NCH + c + 1) * 128],
                                                   rhs=xns[ti][:, c * TT:(c + 1) * TT], start=(c == 0), stop=(c == NCH - 1)),
                                reads=[wt, xns[ti]], writes=[pu])
                    sg = K.sg[ti]
                    K.act.op(lambda e: e.activation(out=sg[:, :], in_=pg[:, :], func=AF.Silu), reads=[pg], writes=[sg])
                    hd = K.hid[i][ti]
                    K.dve.op(lambda e: e.tensor_tensor(out=hd[:, :], in0=pu[:, :], in1=sg[:, :], op=ALU.mult),
                             reads=[pu, sg], writes=[hd])
            for j in range(getattr(K, 'dbg_ndn', NCH)):
                wt = K.ws.next([K.wspec(Wdn, j * 128, 128, 0, NFC)])
                for ti in range(2):
                    pd = K.P[4 + ti]
                    for i in range(NFC):
                        K.pe.op(lambda e: e.matmul(pd[:, :], lhsT=wt[:, i * 128:(i + 1) * 128], rhs=K.hid[i][ti][:, :],
                                                   start=(i == 0), stop=(i == NFC - 1)),
                                reads=[wt, K.hid[i][ti]], writes=[pd])
                    fo = K.ff[j][ti]
                    K.act.op(lambda e: e.activation(out=fo[:, :], in_=pd[:, :], func=AF.Copy), reads=[pd], writes=[fo])
            for ti, tt in enumerate(tts if getattr(K, 'dbg_post', True) else []):
                K.postnorm_add(l, n_post, tt, [K.ff[j][ti] for j in range(NCH)], ti, half=True)

    def shortconv(self, l):
        K = self
        j_l = l // 2
        Win = K.scin[j_l]
        Wout = K.scout[j_l]

        def cw(k, j):
            col = CV_SCW + (j_l * 3 + k) * NCH + j
            return K.cv[:, col:col + 1]

        for tt in range(NTT):
            xn = K.prenorm(l, 2, tt, tt % 2)
            for j in range(NCH):
                wt = K.ws.next([K.wspec(Win, q * D + j * 128, 128, q * NCH * 128, NCH) for q in range(3)])
                pb, pc, pv = K.P[j % 2], K.P[2 + j % 2], K.P[4 + j % 2]
                for q, pp in enumerate((pb, pc, pv)):
                    for c in range(NCH):
                        K.pe.op(lambda e: e.matmul(pp[:, :], lhsT=wt[:, (q * NCH + c) * 128:(q * NCH + c + 1) * 128],
                                                   rhs=xn[:, c * TT:(c + 1) * TT], start=(c == 0), stop=(c == NCH - 1)),
                                reads=[wt, xn], writes=[pp])
                cs = K.csb[j % 2]
                cvb = K.cvb[j]
                acc = K.acc[j % 2]
                if tt == 0:
                    K.act.op(lambda e: e.activation(out=cvb[:, 0:2], in_=K.cv[:, CV_ZERO:CV_ZERO + 2], func=AF.Copy),
                             reads=[K.cv], writes=[cvb])
                else:
                    K.act.op(lambda e: e.activation(out=cvb[:, 0:2], in_=cvb[:, TT:TT + 2], func=AF.Copy),
                             reads=[cvb], writes=[cvb])
                K.act.op(lambda e: e.activation(out=cs[:, :], in_=pc[:, :], func=AF.Copy), reads=[pc], writes=[cs])
                K.dve.op(lambda e: e.tensor_tensor(out=cvb[:, 2:TT + 2], in0=pv[:, :], in1=cs[:, :], op=ALU.mult),
                         reads=[pv, cs], writes=[cvb])
                K.act.op(lambda e: e.activation(out=acc[:, :], in_=cvb[:, 2:TT + 2], func=AF.Copy, scale=cw(2, j)),
                         reads=[cvb, K.cv], writes=[acc])
                K.dve.op(lambda e: e.scalar_tensor_tensor(out=acc[:, :], in0=cvb[:, 1:TT + 1], scalar=cw(1, j),
                                                          in1=acc[:, :], op0=ALU.mult, op1=ALU.add),
                         reads=[cvb, acc, K.cv], writes=[acc])
                K.dve.op(lambda e: e.scalar_tensor_tensor(out=acc[:, :], in0=cvb[:, 0:TT], scalar=cw(0, j),
                                                          in1=acc[:, :], op0=ALU.mult, op1=ALU.add),
                         reads=[cvb, acc, K.cv], writes=[acc])
                K.dve.op(lambda e: e.tensor_tensor(out=K.ysc[:, j * TT:(j + 1) * TT], in0=pb[:, :], in1=acc[:, :],
                                                   op=ALU.mult),
                         reads=[pb, acc], writes=[K.ysc])
            for jd in range(NCH):
                wt = K.ws.next([K.wspec(Wout, jd * 128, 128, 0, NCH)])
                pd = K.P[6]
                for c in range(NCH):
                    K.pe.op(lambda e: e.matmul(pd[:, :], lhsT=wt[:, c * 128:(c + 1) * 128],
                                               rhs=K.ysc[:, c * TT:(c + 1) * TT], start=(c == 0), stop=(c == NCH - 1)),
                            reads=[wt, K.ysc], writes=[pd])
                fo = K.ff[jd][0]
                K.act.op(lambda e: e.activation(out=fo[:, :], in_=pd[:, :], func=AF.Copy), reads=[pd], writes=[fo])
            K.postnorm_add(l, 3, tt, [K.ff[j][0] for j in range(NCH)], tt % 2, half=False)

    def pg(self, bank, g0, ng=1):
        return self.Pst[bank].view(g0 * 512, ng * 512)

    def gdn(self, l):
        K = self
        j_l = l // 2
        Win = K.gdin[j_l]
        Wout = K.gdout[j_l]
        act, dve, pe = K.act, K.dve, K.pe
        cvc = lambda col: K.cv[:, col:col + 1]

        def gcw(k, ch):
            return cvc(CV_GCW + (j_l * 4 + k) * 32 + ch)

        betaT, gA, gB, etT = K.g_rows
        W = K.g_w
        identb = K.ident_b()
        act.op(lambda e: e.activation(out=K.nexpalog[0:32, 0:1], in_=K.cv[0:32, CV_ALOG + j_l:CV_ALOG + j_l + 1], func=AF.Exp),
               reads=[K.cv], writes=[K.nexpalog])
        dve.op(lambda e: e.tensor_scalar(out=K.nexpalog[0:32, 0:1], in0=K.nexpalog[0:32, 0:1], scalar1=-1.0, scalar2=None,
                                         op0=ALU.mult), reads=[K.nexpalog], writes=[K.nexpalog])
        if hasattr(K, 'dbg_gtt'):
            for t_ in K.g_og + K.g_oT:
                dve.op(lambda e: e.memset(t_[:, :], 0.0), writes=[t_])
        for tt in range(getattr(K, 'dbg_gtt', NTT)):
            xn = K.prenorm(l, 2, tt, 0)
            def _dst2(o0, lo):
                return lambda slot: slot[:, o0:o0 + NCH * 32].rearrange("p (k f) -> p k f", k=NCH)[:, :, lo:lo + 16]
            wsrc = lambda c0: Win[:, c0:c0 + 16].rearrange("(k p) f -> p k f", p=128)
            wt = K.ws.next([K.wspec(Win, 6144, 32, 0, NCH), (_dst2(256, 0), wsrc(6160)), (_dst2(256, 16), wsrc(6144))])
            pb, pa = K.P[5], K.P[6]
            for q, pp in enumerate((pb, pa)):
                for c in range(NCH):
                    pe.op(lambda e: e.matmul(pp[0:32, :], lhsT=wt[:, q * 256 + c * 32:q * 256 + c * 32 + 32],
                                             rhs=xn[:, c * TT:(c + 1) * TT], start=(c == 0), stop=(c == NCH - 1)),
                          reads=[wt, xn], writes=[pp])
            act.op(lambda e: e.activation(out=betaT[0:32, :], in_=pb[0:32, :], func=AF.Sigmoid), reads=[pb], writes=[betaT])
            act.op(lambda e: e.activation(out=gA[0:32, :], in_=pa[0:32, :], func=AF.Exp,
                                          bias=K.cv[0:32, CV_DTB + j_l:CV_DTB + j_l + 1]), reads=[pa, K.cv], writes=[gA])
            act.op(lambda e: e.activation(out=gA[0:32, :], in_=gA[0:32, :], func=AF.Ln, bias=K.cv[0:32, CV_ONE:CV_ONE + 1]),
                   reads=[gA, K.cv], writes=[gA])
            dve.op(lambda e: e.tensor_scalar(out=gB[0:32, :], in0=gA[0:32, :], scalar1=K.nexpalog[0:32, 0:1], scalar2=None,
                                             op0=ALU.mult), reads=[gA, K.nexpalog], writes=[gB])
            if getattr(K, 'dbg_stop', 0) == 20:
                return
            src, dst = gB, gA
            sh = 1
            while sh < 128:
                s3 = src[0:32, :].rearrange("p (n c) -> p n c", n=4)
                d3 = dst[0:32, :].rearrange("p (n c) -> p n c", n=4)
                dve.op(lambda e: e.tensor_tensor(out=d3[:, :, sh:128], in0=s3[:, :, sh:128], in1=s3[:, :, 0:128 - sh],
                                                 op=ALU.add), reads=[src], writes=[dst])
                act.op(lambda e: e.activation(out=d3[:, :, 0:sh], in_=s3[:, :, 0:sh], func=AF.Copy), reads=[src], writes=[dst])
                src, dst = dst, src
                sh *= 2
            gcT = src
            other = dst
            if getattr(K, 'dbg_stop', 0) == 21:
                return
            for n in range(4):
                glc = gcT[0:32, n * 128 + 127:n * 128 + 128]
                act.op(lambda e: e.activation(out=etT[0:32, n * 128:(n + 1) * 128], in_=gcT[0:32, n * 128:(n + 1) * 128],
                                              func=AF.Exp, scale=-1.0, bias=glc), reads=[gcT], writes=[etT])
                act.op(lambda e: e.activation(out=K.g_egl[0:32, n:n + 1], in_=glc, func=AF.Exp), reads=[gcT], writes=[K.g_egl])
            for n in range(4):
                dve.op(lambda e: e.tensor_scalar(out=K.g_R[0:32, n * 16:(n + 1) * 16], in0=K.cm[0:32, CM_IDENT:CM_IDENT + 16],
                                                 scalar1=K.g_egl[0:32, n:n + 1], scalar2=None, op0=ALU.mult),
                       reads=[K.cm, K.g_egl], writes=[K.g_R])
            if getattr(K, 'dbg_stop', 0) == 22:
                return
            p3 = K.P[3]
            pe.op(lambda e: e.matmul(p3[:, 256:320], lhsT=K.cm[0:32, CM_ONESF:CM_ONESF + 128], rhs=K.g_R[0:32, 0:64],
                                     start=True, stop=True), reads=[K.cm, K.g_R], writes=[p3])
            for n in range(4):
                for q, srcT in enumerate((gcT, betaT, etT)):
                    pe.op(lambda e: e.matmul(p3[:, n * 48 + q * 16:n * 48 + q * 16 + 16],
                                             lhsT=srcT[0:32, n * 128:(n + 1) * 128], rhs=K.cm[0:32, CM_IDENT:CM_IDENT + 16],
                                             start=True, stop=True), reads=[srcT, K.cm], writes=[p3])
            act.op(lambda e: e.activation(out=K.g_scal[:, 0:192], in_=p3[:, 0:192], func=AF.Copy), reads=[p3], writes=[K.g_scal])
            act.op(lambda e: e.activation(out=K.g_eglb[:, 0:64], in_=p3[:, 256:320], func=AF.Copy), reads=[p3], writes=[K.g_eglb])
            dve.op(lambda e: e.tensor_scalar(out=K.g_nscal[:, 0:192], in0=K.g_scal[:, 0:192], scalar1=-1.0, scalar2=None,
                                             op0=ALU.mult), reads=[K.g_scal], writes=[K.g_nscal])
            for n in range(4):
                o = n * 48
                act.op(lambda e: e.activation(out=K.g_scal2[:, o:o + 16], in_=K.g_scal[:, o:o + 16], func=AF.Exp),
                       reads=[K.g_scal], writes=[K.g_scal2])
                dve.op(lambda e: e.tensor_tensor(out=K.g_scal2[:, o + 16:o + 32], in0=K.g_scal2[:, o:o + 16],
                                                 in1=K.g_scal[:, o + 16:o + 32], op=ALU.mult),
                       reads=[K.g_scal, K.g_scal2], writes=[K.g_scal2])
            if getattr(K, 'dbg_stop', 0) == 23:
                return
            sc_gc = lambda n, h: K.g_scal[:, n * 48 + h:n * 48 + h + 1]
            sc_beta = lambda n, h: K.g_scal[:, n * 48 + 16 + h:n * 48 + 16 + h + 1]
            sc_et = lambda n, h: K.g_scal[:, n * 48 + 32 + h:n * 48 + 32 + h + 1]
            sc_nbeta = lambda n, h: K.g_nscal[:, n * 48 + 16 + h:n * 48 + 16 + h + 1]
            sc_bge = lambda n, h: K.g_scal2[:, n * 48 + 16 + h:n * 48 + 16 + h + 1]

            def proj(wt, q, bank):
                pp = K.P[bank]
                for c in range(NCH):
                    pe.op(lambda e: e.matmul(pp[:, :], lhsT=wt[:, (q * NCH + c) * 128:(q * NCH + c + 1) * 128],
                                             rhs=xn[:, c * TT:(c + 1) * TT], start=(c == 0), stop=(c == NCH - 1)),
                          reads=[wt, xn], writes=[pp])
                return pp

            def conv_silu(pp, ch, slot):
                pre = K.g_pre[slot]
                acc = K.g_acc[slot]
                hl = K.g_halo[:, ch * 3:ch * 3 + 3]
                if tt == 0:
                    act.op(lambda e: e.activation(out=pre[:, 0:3], in_=K.cv[:, CV_ZERO:CV_ZERO + 3], func=AF.Copy),
                           reads=[K.cv], writes=[pre])
                else:
                    act.op(lambda e: e.activation(out=pre[:, 0:3], in_=hl, func=AF.Copy), reads=[K.g_halo], writes=[pre])
                act.op(lambda e: e.activation(out=pre[:, 3:TT + 3], in_=pp[:, :], func=AF.Copy), reads=[pp], writes=[pre])
                act.op(lambda e: e.activation(out=hl, in_=pre[:, TT:TT + 3], func=AF.Copy), reads=[pre], writes=[K.g_halo])
                act.op(lambda e: e.activation(out=acc[:, :], in_=pre[:, 3:TT + 3], func=AF.Copy, scale=gcw(3, ch)),
                       reads=[pre, K.cv], writes=[acc])
                for k in (2, 1, 0):
                    dve.op(lambda e: e.scalar_tensor_tensor(out=acc[:, :], in0=pre[:, k:TT + k], scalar=gcw(k, ch),
                                                            in1=acc[:, :], op0=ALU.mult, op1=ALU.add),
                           reads=[pre, acc, K.cv], writes=[acc])
                act.op(lambda e: e.activation(out=acc[:, :], in_=acc[:, :], func=AF.Silu), reads=[acc], writes=[acc])
                return acc

            def l2n(acc, dst, qscale):
                sq = K.sqb[0]
                p3 = K.P[3]
                rin = K.g_rinv
                act.op(lambda e: e.activation(out=sq[:, :], in_=acc[:, :], func=AF.Square), reads=[acc], writes=[sq])
                pe.op(lambda e: e.matmul(p3[:, :], lhsT=K.ones_b(), rhs=sq[:, :], start=True, stop=True),
                      reads=[sq, K.cmb], writes=[p3])
                sc = 128.0 if qscale else 1.0
                col = CV_L2Q if qscale else CV_L2
                act.op(lambda e: e.activation(out=rin[:, :], in_=p3[:, :], func=AF.Sqrt, scale=sc, bias=cvc(col)),
                       reads=[p3, K.cv], writes=[rin])
                dve.op(lambda e: e.reciprocal(out=rin[:, :], in_=rin[:, :]), reads=[rin], writes=[rin])
                dve.op(lambda e: e.tensor_tensor(out=dst[:, :], in0=acc[:, :], in1=rin[:, :], op=ALU.mult),
                       reads=[acc, rin], writes=[dst])

            for kh in range(getattr(K, 'dbg_gkh', 8)):
                wtA = K.ws.next([K.wspec(Win, kh * 128, 128, 0, NCH), K.wspec(Win, 1024 + kh * 128, 128, NCH * 128, NCH),
                                 K.wspec(Win, 2048 + (2 * kh) * 128, 128, 2 * NCH * 128, NCH)])
                pp = proj(wtA, 0, 0)
                l2n(conv_silu(pp, kh, 0), K.g_qT, True)
                if getattr(K, 'dbg_stopq', False):
                    return
                pp = proj(wtA, 1, 1)
                l2n(conv_silu(pp, 8 + kh, 1), K.g_kT, False)
                if getattr(K, 'dbg_stopk', False):
                    return
                pp = proj(wtA, 2, 2)
                a0 = conv_silu(pp, 16 + 2 * kh, 0)
                if getattr(K, 'dbg_stop', 0) == 30:
                    return
                dve.op(lambda e: e.tensor_copy(out=K.g_vT[0][:, :], in_=a0[:, :]), reads=[a0], writes=[K.g_vT[0]])
                if getattr(K, 'dbg_stop', 0) == 31:
                    return
                wtB = K.ws.next([K.wspec(Win, 2048 + (2 * kh + 1) * 128, 128, 0, NCH),
                                 K.wspec(Win, 4096 + (2 * kh) * 128, 128, NCH * 128, NCH),
                                 K.wspec(Win, 4096 + (2 * kh + 1) * 128, 128, 2 * NCH * 128, NCH)])
                pp = proj(wtB, 0, getattr(K, 'dbg_b1', 0))
                a1 = conv_silu(pp, 16 + 2 * kh + 1, getattr(K, 'dbg_s1', 1))
                dve.op(lambda e: e.tensor_copy(out=K.g_vT[1][:, :], in_=a1[:, :]), reads=[a1], writes=[K.g_vT[1]])
                if getattr(K, 'dbg_stop', 0) == 32:
                    return
                for j in range(2):
                    pp = proj(wtB, 1 + j, 1 + j)
                    act.op(lambda e: e.activation(out=K.g_zs[j][:, :], in_=pp[:, :], func=AF.Silu), reads=[pp], writes=[K.g_zs[j]])
                qT, kT = K.g_qT, K.g_kT
                if getattr(K, 'dbg_stop', 0) == 1:
                    return
                def head_chain(j):
                    h = 2 * kh + j
                    vT = K.g_vT[j]
                    S, Sb = K.g_S[h], K.g_Sb[h]
                    oTs = K.g_oT[j]
                    W = K.g_wj[j]
                    bD, bA, bC = ((0, 1, 2), (5, 6, 7))[j]
                    oth = (other, betaT)[j]
                    expgb, qg, rin = K.g_expgbj[j], K.g_qgj[j], K.g_rinvj[j]
                    if tt == 0:
                        dve.op(lambda e: e.memset(S[:, :], 0.0), writes=[S])
                        yield
                        dve.op(lambda e: e.memset(Sb[:, :], 0.0), writes=[Sb])
                        yield
                    p4 = K.P[(4, 3)[j]]
                    dve.op(lambda e: e.tensor_scalar(out=oth[0:32, :], in0=gcT[0:32, :],
                                                     scalar1=K.cm[0:32, CM_IDENT + h:CM_IDENT + h + 1], scalar2=None,
                                                     op0=ALU.mult), reads=[gcT, K.cm], writes=[oth])
                    yield
                    pe.op(lambda e: e.matmul(p4[:, :], lhsT=K.cm[0:32, CM_ONESF:CM_ONESF + 128],
                                             rhs=oth[0:32, :], start=True, stop=True), reads=[K.cm, oth], writes=[p4])
                    yield
                    act.op(lambda e: e.activation(out=expgb[:, :], in_=p4[:, :], func=AF.Exp), reads=[p4], writes=[expgb])
                    yield
                    dve.op(lambda e: e.tensor_tensor(out=qg[:, :], in0=qT[:, :], in1=expgb[:, :], op=ALU.mult),
                           reads=[qT, expgb], writes=[qg])
                    yield
                    for n in range(getattr(K, 'dbg_gn', 4)):
                        cs = slice(n * 128, (n + 1) * 128)
                        Gb = p4.sub(n * 128, (n + 1) * 128)
                        KKp, QKp, Xp = K.pg(bD, 0), K.pg(bD, 1), K.pg(bD, 2, 2)
                        wSp, dSp = K.pg(bD, 0), K.pg(bD, 1)
                        ktp, vtp, cM, oTp = K.pg(bA, 0), K.pg(bA, 1), K.pg(bA, 2), K.pg(bA, 3)
                        cA = K.pg(bC, 0)
                        pe.op(lambda e: e.matmul(KKp[:, :], lhsT=kT[:, cs], rhs=kT[:, cs], start=True, stop=True),
                              reads=[kT], writes=[KKp])
                        yield
                        pe.op(lambda e: e.matmul(QKp[:, :], lhsT=kT[:, cs], rhs=qT[:, cs], start=True, stop=True),
                              reads=[kT, qT], writes=[QKp])
                        yield
                        pe.op(lambda e: e.matmul(ktp[:, :], lhsT=kT[:, cs], rhs=identb, start=True, stop=True),
                              reads=[kT, K.cmb], writes=[ktp])
                        yield
                        pe.op(lambda e: e.matmul(vtp[:, :], lhsT=vT[:, cs], rhs=identb, start=True, stop=True),
                              reads=[vT, K.cmb], writes=[vtp])
                        yield
                        dve.op(lambda e: e.scalar_tensor_tensor(out=W["y1"][:, :], in0=Gb[:, :], scalar=sc_gc(n, h),
                                                                in1=K.cm[:, CM_PMASK:CM_PMASK + 128], op0=ALU.subtract,
                                                                op1=ALU.add), reads=[Gb, K.g_scal, K.cm], writes=[W["y1"]])
                        yield
                        act.op(lambda e: e.activation(out=W["Dst"][:, :], in_=W["y1"][:, :], func=AF.Exp, scale=-1.0),
                               reads=[W["y1"]], writes=[W["Dst"]])
                        yield
                        dve.op(lambda e: e.scalar_tensor_tensor(out=W["nA0"][:, :], in0=KKp[:, :], scalar=sc_nbeta(n, h),
                                                                in1=W["Dst"][:, :], op0=ALU.mult, op1=ALU.mult),
                               reads=[KKp, K.g_nscal, W["Dst"]], writes=[W["nA0"]])
                        yield
                        dve.op(lambda e: e.scalar_tensor_tensor(out=W["y2"][:, :], in0=Gb[:, :], scalar=sc_gc(n, h),
                                                                in1=K.cm[:, CM_NMASK:CM_NMASK + 128], op0=ALU.subtract,
                                                                op1=ALU.add), reads=[Gb, K.g_scal, K.cm], writes=[W["y2"]])
                        yield
                        act.op(lambda e: e.activation(out=W["Dti"][:, :], in_=W["y2"][:, :], func=AF.Exp),
                               reads=[W["y2"]], writes=[W["Dti"]])
                        yield
                        dve.op(lambda e: e.tensor_tensor(out=W["qkT"][:, :], in0=QKp[:, :], in1=W["Dti"][:, :], op=ALU.mult),
                               reads=[QKp, W["Dti"]], writes=[W["qkT"]])
                        yield
                        pe.op(lambda e: e.matmul(cM[:, :], lhsT=W["nA0"][:, :], rhs=identb, start=True, stop=True),
                              reads=[W["nA0"], K.cmb], writes=[cM])
                        yield
                        act.op(lambda e: e.activation(out=W["nM0"][:, :], in_=cM[:, :], func=AF.Copy), reads=[cM], writes=[W["nM0"]])
                        yield
                        Bv = W["B"]
                        act.op(lambda e: e.activation(out=Bv[:, 0:128], in_=vtp[:, :], func=AF.Copy, scale=sc_beta(n, h)),
                               reads=[vtp, K.g_scal], writes=[Bv])
                        yield
                        act.op(lambda e: e.activation(out=Bv[:, 128:256], in_=ktp[:, :], func=AF.Copy, scale=sc_bge(n, h)),
                               reads=[ktp, K.g_scal2], writes=[Bv])
                        yield
                        act.op(lambda e: e.activation(out=W["ktl"][:, :], in_=ktp[:, :], func=AF.Copy, scale=sc_et(n, h)),
                               reads=[ktp, K.g_scal], writes=[W["ktl"]])
                        yield
                        pe.op(lambda e: e.matmul(Xp[:, :], lhsT=identb, rhs=Bv[:, :], start=True, stop=True),
                              reads=[Bv, K.cmb], writes=[Xp])
                        yield
                        pe.op(lambda e: e.matmul(Xp[:, :], lhsT=W["nM0"][:, :], rhs=Bv[:, :], start=False, stop=True, skip_group_check=True),
                              reads=[Bv, W["nM0"]], writes=[Xp])
                        yield
                        Ap, Mp = W["nA0"], W["nM0"]
                        for k in range(1, 7):
                            Mk = W["Mk"][k % 2]
                            pe.op(lambda e: e.matmul(cM[:, :], lhsT=Ap[:, :], rhs=Mp[:, :], start=True, stop=True),
                                  reads=[Ap, Mp], writes=[cM])
                            yield
                            act.op(lambda e: e.activation(out=Mk[:, :], in_=cM[:, :], func=AF.Copy), reads=[cM], writes=[Mk])
                            yield
                            if k < 6:
                                Ak = W["Ak"][k % 2]
                                pe.op(lambda e: e.matmul(cA[:, :], lhsT=Mp[:, :], rhs=Ap[:, :], start=True, stop=True),
                                      reads=[Ap, Mp], writes=[cA])
                                yield
                                dve.op(lambda e: e.tensor_copy(out=Ak[:, :], in_=cA[:, :]), reads=[cA], writes=[Ak])
                                yield
                            else:
                                Ak = None
                            Xb = W["Xbf"][k % 2]
                            dve.op(lambda e: e.tensor_copy(out=Xb[:, :], in_=Xp[:, :]), reads=[Xp], writes=[Xb])
                            yield
                            pe.op(lambda e: e.matmul(Xp[:, :], lhsT=Mk[:, :], rhs=Xb[:, :], start=False, stop=True, skip_group_check=True),
                                  reads=[Mk, Xb], writes=[Xp])
                            yield
                            Ap, Mp = Ak, Mk
                        dve.op(lambda e: e.tensor_copy(out=W["u"][:, :], in_=Xp[:, 0:128]), reads=[Xp], writes=[W["u"]])
                        yield
                        dve.op(lambda e: e.tensor_copy(out=W["wbf"][:, :], in_=Xp[:, 128:256]), reads=[Xp], writes=[W["wbf"]])
                        yield
                        pe.op(lambda e: e.matmul(cM[:, :], lhsT=W["wbf"][:, :], rhs=identb, start=True, stop=True),
                              reads=[W["wbf"], K.cmb], writes=[cM])
                        yield
                        act.op(lambda e: e.activation(out=W["wT"][:, :], in_=cM[:, :], func=AF.Copy), reads=[cM], writes=[W["wT"]])
                        yield
                        pe.op(lambda e: e.matmul(wSp[:, :], lhsT=W["wT"][:, :], rhs=Sb[:, :], start=True, stop=True),
                              reads=[W["wT"], Sb], writes=[wSp])
                        yield
                        dve.op(lambda e: e.tensor_tensor(out=W["vnew"][:, :], in0=W["u"][:, :], in1=wSp[:, :], op=ALU.subtract),
                               reads=[W["u"], wSp], writes=[W["vnew"]])
                        yield
                        pe.op(lambda e: e.matmul(oTp[:, :], lhsT=Sb[:, :], rhs=qg[:, cs], start=True, stop=False),
                              reads=[Sb, qg], writes=[oTp])
                        yield
                        pe.op(lambda e: e.matmul(oTp[:, :], lhsT=W["vnew"][:, :], rhs=W["qkT"][:, :], start=False, stop=True),
                              reads=[W["vnew"], W["qkT"]], writes=[oTp])
                        yield
                        act.op(lambda e: e.activation(out=oTs[:, cs], in_=oTp[:, :], func=AF.Copy), reads=[oTp], writes=[oTs])
                        yield
                        pe.op(lambda e: e.matmul(dSp[:, :], lhsT=W["ktl"][:, :], rhs=W["vnew"][:, :], start=True, stop=True),
                              reads=[W["ktl"], W["vnew"]], writes=[dSp])
                        yield
                        dve.op(lambda e: e.scalar_tensor_tensor(out=S[:, :], in0=S[:, :],
                                                                scalar=K.g_eglb[:, n * 16 + h:n * 16 + h + 1],
                                                                in1=dSp[:, :], op0=ALU.mult, op1=ALU.add),
                               reads=[S, K.g_eglb, dSp], writes=[S])
                        yield
                        act.op(lambda e: e.activation(out=Sb[:, :], in_=S[:, :], func=AF.Copy), reads=[S], writes=[Sb])
                        yield
                    sq = K.sqb[j]
                    p3 = K.P[bD]
                    act.op(lambda e: e.activation(out=sq[:, :], in_=oTs[:, :], func=AF.Square), reads=[oTs], writes=[sq])
                    yield
                    pe.op(lambda e: e.matmul(p3[:, :], lhsT=K.ones_b(), rhs=sq[:, :], start=True, stop=True),
                          reads=[sq, K.cmb], writes=[p3])
                    yield
                    act.op(lambda e: e.activation(out=rin[:, :], in_=p3[:, :], func=AF.Sqrt, scale=1.0 / 128,
                                                  bias=cvc(CV_EPS)), reads=[p3, K.cv], writes=[rin])
                    yield
                    dve.op(lambda e: e.reciprocal(out=rin[:, :], in_=rin[:, :]), reads=[rin], writes=[rin])
                    yield
                    dve.op(lambda e: e.scalar_tensor_tensor(out=oTs[:, :], in0=oTs[:, :], scalar=cvc(CV_GNW + j_l),
                                                            in1=rin[:, :], op0=ALU.mult, op1=ALU.mult),
                           reads=[oTs, rin, K.cv], writes=[oTs])
                    yield
                    dve.op(lambda e: e.tensor_tensor(out=K.g_og[h][:, :], in0=oTs[:, :], in1=K.g_zs[j][:, :], op=ALU.mult),
                           reads=[oTs, K.g_zs[j]], writes=[K.g_og[h]])
                    yield
                chains = [head_chain(0), head_chain(1)]
                while chains:
                    for ch_ in list(chains):
                        try:
                            next(ch_)
                        except StopIteration:
                            chains.remove(ch_)
            for jd in range(NCH):
                wt = K.ws.next([K.wspec(Wout, jd * 128, 128, 0, 16)])
                pd = K.P[jd % 3]
                for hh in range(16):
                    pe.op(lambda e: e.matmul(pd[:, :], lhsT=wt[:, hh * 128:(hh + 1) * 128], rhs=K.g_og[hh][:, :],
                                             start=(hh == 0), stop=(hh == 15)), reads=[wt, K.g_og[hh]], writes=[pd])
                fo = K.ff[jd][0]
                act.op(lambda e: e.activation(out=fo[:, :], in_=pd[:, :], func=AF.Copy), reads=[pd], writes=[fo])
            K.postnorm_add(l, 3, tt, [K.ff[j][0] for j in range(NCH)], 0, half=False)


CV_G = 0
CV_SCW = CV_G + DEPTH * 6 * NCH
CV_ZERO = CV_SCW + 2 * 3 * NCH
CV_EPS = CV_ZERO + 4
CV_ONE = CV_EPS + 1
CV_L2 = CV_ONE + 1
CV_L2Q = CV_L2 + 1
CV_GCW = CV_L2Q + 1
CV_GNW = CV_GCW + 2 * 4 * 32
CV_ALOG = CV_GNW + 2
CV_DTB = CV_ALOG + 2
CV_W = CV_DTB + 2
CM_IDENT = 0
CM_ONES = 128
CM_PMASK = 256
CM_NMASK = 384
CM_ONESF = 512
CM_W = 640
BIG = 1.0e8


def pack_consts(inputs):
    norm_g, sc_conv_w = inputs["norm_g"], inputs["sc_conv_w"]
    f32 = np.float32
    cvec = np.zeros((128, CV_W), f32)
    cvec[:, CV_G:CV_G + DEPTH * 6 * NCH] = np.asarray(norm_g, f32).reshape(DEPTH * 6, NCH, 128).transpose(2, 0, 1).reshape(128, -1)
    cvec[:, CV_SCW:CV_SCW + 2 * 3 * NCH] = np.asarray(sc_conv_w, f32).reshape(2 * 3, NCH, 128).transpose(2, 0, 1).reshape(128, -1)
    cvec[:, CV_EPS] = RMS_EPS
    cvec[:, CV_ONE] = 1.0
    cvec[:, CV_L2] = 1e-6
    cvec[:, CV_L2Q] = 128e-6
    cvec[:, CV_GCW:CV_GCW + 256] = np.asarray(inputs["gdn_conv_w"], f32).reshape(2 * 4, 32, 128).transpose(2, 0, 1).reshape(128, -1)
    cvec[:, CV_GNW:CV_GNW + 2] = np.asarray(inputs["gdn_norm_w"], f32).T
    cvec[0:16, CV_ALOG:CV_ALOG + 2] = np.asarray(inputs["gdn_a_log"], f32).T
    cvec[0:16, CV_DTB:CV_DTB + 2] = np.asarray(inputs["gdn_dt_bias"], f32).T
    cmat = np.zeros((128, CM_W), f32)
    cmat[:, CM_IDENT:CM_IDENT + 128] = np.eye(128, dtype=f32)
    cmat[:, CM_ONES:CM_ONES + 128] = 1.0
    p = np.arange(128)[:, None]
    f = np.arange(128)[None, :]
    cmat[:, CM_PMASK:CM_PMASK + 128] = np.where(p > f, 0.0, BIG)
    cmat[:, CM_NMASK:CM_NMASK + 128] = np.where(f >= p, 0.0, -BIG)
    cmat[:, CM_ONESF:CM_ONESF + 128] = 1.0
    return cvec, cmat


_CACHE = {}


def get_nc(nseq, layers):
    key = (nseq, tuple(layers))
    if key not in _CACHE:
        _CACHE[key] = Builder(nseq, list(layers)).build()
    return _CACHE[key]


def run(inputs, x, layers, ncores):
    nseq = x.shape[0] // ncores
    nc = get_nc(nseq, layers)
    cvec, cmat = pack_consts(inputs)
    shared = {"cvec": cvec, "cmat": cmat}
    for k in ("ffn_w_gate_up", "ffn_w_down", "sc_w_in", "sc_w_out", "gdn_w_in", "gdn_w_out"):
        shared[k] = np.ascontiguousarray(inputs[k], dtype=np.float32)
    in_maps = []
    for i in range(ncores):
        m = dict(shared)
        m["x"] = np.ascontiguousarray(x[i * nseq:(i + 1) * nseq])
        in_maps.append(m)
    res = run_bass_kernel_spmd(nc, in_maps, core_ids=list(range(ncores)))
    return np.concatenate([r["out"] for r in res.results], axis=0)


def kernel(**inputs):
    x = np.asarray(inputs["x"], dtype=np.float32)
    return run(inputs, x, [0, 1, 2, 3], 8)
```
